# Optimizing a Trainium2 kernel written in Bass

```python
import math
import jax
import jax.numpy as jnp
from jax import lax
import numpy as np

D_MODEL = 1024
BATCH = 2
SEQ = 8192
DEPTH = 2

MLA_HEADS = 8
MLA_Q_RANK = 256
MLA_KV_RANK = 128
MLA_NOPE_DIM = 64
MLA_ROPE_DIM = 32
MLA_V_DIM = 64
MLA_QK_DIM = MLA_NOPE_DIM + MLA_ROPE_DIM
ROPE_THETA = 10000.0
SWA_HEADS = 8
SWA_KV_HEADS = 2
SWA_REP = SWA_HEADS // SWA_KV_HEADS
SWA_HEAD_DIM = 64
WINDOW = 128
BLOCK = 128
REL_BUCKETS = 32
REL_MAX_DIST = 128
A_COLS = MLA_Q_RANK + MLA_KV_RANK + MLA_ROPE_DIM
B_COLS = (SWA_HEADS + 2 * SWA_KV_HEADS) * SWA_HEAD_DIM
IN0_COLS = A_COLS + B_COLS
MIX0_WIDTH = MLA_HEADS * MLA_V_DIM + SWA_HEADS * SWA_HEAD_DIM
LRU_WIDTH = D_MODEL
LRU_BLOCKS = 8
LRU_BW = LRU_WIDTH // LRU_BLOCKS
LRU_C = 8.0
CONV_WIDTH = 4
CONV_LEFT = 2
N_EXPERTS = 16
N_GROUPS = 4
EXPERTS_PER_GROUP = N_EXPERTS // N_GROUPS
TOP_K = 2
GROUP_SCORE_K = 2
D_EXPERT = 512
ALPHA = (2.0 * DEPTH) ** 0.25
BETA = (8.0 * DEPTH) ** -0.25
LN_EPS = 1e-5
RMS_EPS = 1e-6

kernel_name = 'hybrid_mla_swa_rglru_grouped_moe_encoder'


def layer_norm(x, g, b):
    xf = x.astype(jnp.float32)
    mu = jnp.mean(xf, -1, keepdims=True)
    var = jnp.mean(jnp.square(xf - mu), -1, keepdims=True)
    return ((xf - mu) * lax.rsqrt(var + LN_EPS) * g.astype(jnp.float32) + b.astype(jnp.float32)).astype(x.dtype)


def rms_norm(x, g):
    xf = x.astype(jnp.float32)
    return (xf * lax.rsqrt(jnp.mean(jnp.square(xf), -1, keepdims=True) + RMS_EPS) * g.astype(jnp.float32)).astype(x.dtype)


def rotary_tables(seq, dtype):
    half = MLA_ROPE_DIM // 2
    inv_freq = ROPE_THETA ** (-jnp.arange(half, dtype=jnp.float32) / half)
    ang = jnp.arange(seq, dtype=jnp.float32)[:, None] * inv_freq[None, :]
    return jnp.cos(ang).astype(dtype), jnp.sin(ang).astype(dtype)


def apply_rope(x, cos, sin):
    x1, x2 = jnp.split(x, 2, axis=-1)
    return jnp.concatenate([x1 * cos - x2 * sin, x1 * sin + x2 * cos], axis=-1)


def mla_attention(q_lat, kv_lat, k_rope, q_norm, w_uq, kv_norm, w_ukv, cos, sin):
    bsz, seq, _ = q_lat.shape
    q = (rms_norm(q_lat, q_norm) @ w_uq).reshape(bsz, seq, MLA_HEADS, MLA_QK_DIM)
    q = jnp.concatenate([q[..., :MLA_NOPE_DIM],
                         apply_rope(q[..., MLA_NOPE_DIM:], cos[:, None, :], sin[:, None, :])], axis=-1)
    kv = (rms_norm(kv_lat, kv_norm) @ w_ukv).reshape(bsz, seq, MLA_HEADS, MLA_NOPE_DIM + MLA_V_DIM)
    k_nope, v = kv[..., :MLA_NOPE_DIM], kv[..., MLA_NOPE_DIM:]
    k_pe = apply_rope(k_rope, cos, sin)
    k = jnp.concatenate([k_nope, jnp.broadcast_to(k_pe[:, :, None, :], (bsz, seq, MLA_HEADS, MLA_ROPE_DIM))], axis=-1)
    scale = MLA_QK_DIM ** -0.5
    n_blk = seq // BLOCK
    q_blocks = jnp.moveaxis(q.reshape(bsz, n_blk, BLOCK, MLA_HEADS, MLA_QK_DIM), 1, 0)

    def attend(qb):
        s = jnp.einsum('bqhd,bkhd->bhqk', qb, k).astype(jnp.float32) * scale
        p = jax.nn.softmax(s, axis=-1).astype(v.dtype)
        return jnp.einsum('bhqk,bkhd->bqhd', p, v)

    o = lax.map(attend, q_blocks)
    return jnp.moveaxis(o, 0, 1).reshape(bsz, seq, MLA_HEADS * MLA_V_DIM)


def t5_bucket(rel):
    n_side = REL_BUCKETS // 2
    max_exact = n_side // 2
    dist = jnp.abs(rel)
    far = max_exact + (jnp.log(jnp.maximum(dist, 1).astype(jnp.float32) / max_exact)
                       / math.log(REL_MAX_DIST / max_exact) * (n_side - max_exact)).astype(jnp.int32)
    far = jnp.minimum(far, n_side - 1)
    return jnp.where(rel > 0, n_side, 0) + jnp.where(dist < max_exact, dist, far)


def windowed_gqa(q, k, v, sinks, rel_bias):
    bsz, seq, _ = q.shape
    n_blk = seq // BLOCK
    qb = q.reshape(bsz, n_blk, BLOCK, SWA_KV_HEADS, SWA_REP, SWA_HEAD_DIM)

    def band(t):
        tp = jnp.pad(t.reshape(bsz, seq, SWA_KV_HEADS, SWA_HEAD_DIM), ((0, 0), (BLOCK, BLOCK), (0, 0), (0, 0)))
        tp = tp.reshape(bsz, n_blk + 2, BLOCK, SWA_KV_HEADS, SWA_HEAD_DIM)
        return jnp.concatenate([tp[:, :-2], tp[:, 1:-1], tp[:, 2:]], axis=2)

    kb, vb = band(k), band(v)
    s = jnp.einsum('bnqgrd,bnkgd->bgrnqk', qb, kb).astype(jnp.float32) * SWA_HEAD_DIM ** -0.5
    q_off = jnp.arange(BLOCK)
    k_off = jnp.arange(3 * BLOCK)
    rel = k_off[None, :] - BLOCK - q_off[:, None]
    bias = rel_bias.astype(jnp.float32)[t5_bucket(rel)]
    bias = jnp.transpose(bias, (2, 0, 1)).reshape(SWA_KV_HEADS, SWA_REP, 1, BLOCK, 3 * BLOCK)
    key_pos = (jnp.arange(n_blk)[:, None] - 1) * BLOCK + k_off[None, :]
    valid = (jnp.abs(rel) <= WINDOW)[None] & ((key_pos >= 0) & (key_pos < seq))[:, None, :]
    s = jnp.where(valid, s + bias, -jnp.inf)
    sink = jnp.broadcast_to(sinks.astype(jnp.float32).reshape(1, SWA_KV_HEADS, SWA_REP, 1, 1, 1), s.shape[:-1] + (1,))
    p = jax.nn.softmax(jnp.concatenate([s, sink], axis=-1), axis=-1)[..., :-1].astype(v.dtype)
    o = jnp.einsum('bgrnqk,bnkgd->bnqgrd', p, vb)
    return o.reshape(bsz, seq, SWA_HEADS * SWA_HEAD_DIM)


def mixer_attention_pair(x, w_in, q_norm, w_uq, kv_norm, w_ukv, sinks, w_out, rel_bias, cos, sin):
    proj = x @ w_in
    offs = np.cumsum([MLA_Q_RANK, MLA_KV_RANK, MLA_ROPE_DIM,
                      SWA_HEADS * SWA_HEAD_DIM, SWA_KV_HEADS * SWA_HEAD_DIM])
    q_lat, kv_lat, k_rope, q_b, k_b, v_b = jnp.split(proj, [int(o) for o in offs], axis=-1)
    out_a = mla_attention(q_lat, kv_lat, k_rope, q_norm, w_uq, kv_norm, w_ukv, cos, sin)
    out_b = windowed_gqa(q_b, k_b, v_b, sinks, rel_bias)
    return jnp.concatenate([out_a, out_b], axis=-1) @ w_out


def block_diag_linear(x, w, b):
    xb = x.reshape(x.shape[:-1] + (LRU_BLOCKS, LRU_BW))
    return (jnp.einsum('bsnc,ncd->bsnd', xb, w) + b).reshape(x.shape)


def rg_lru(xc, w_a, b_a, w_x, b_x, lam, reverse):
    f32 = jnp.float32
    r = jax.nn.sigmoid(block_diag_linear(xc, w_a.astype(f32), b_a.astype(f32)))
    i = jax.nn.sigmoid(block_diag_linear(xc, w_x.astype(f32), b_x.astype(f32)))
    log_a = LRU_C * r * jax.nn.log_sigmoid(lam.astype(f32))
    a = jnp.exp(log_a)
    b = jnp.sqrt(-jnp.expm1(2.0 * log_a)) * (i * xc)

    def combine(c1, c2):
        a1, b1 = c1
        a2, b2 = c2
        return a1 * a2, a2 * b1 + b2

    _, h = lax.associative_scan(combine, (a, b), reverse=reverse, axis=1)
    return h


def mixer_rglru(x, w_in, conv_w, conv_b, wa_f, ba_f, wx_f, bx_f, lam_f,
                wa_b, ba_b, wx_b, bx_b, lam_b, w_out):
    seq = x.shape[1]
    gate, xr = jnp.split(x @ w_in, 2, axis=-1)
    xp = jnp.pad(xr, ((0, 0), (CONV_LEFT, CONV_WIDTH - 1 - CONV_LEFT), (0, 0)))
    xc = conv_b + sum(xp[:, k:k + seq] * conv_w[k] for k in range(CONV_WIDTH))
    xc = xc.astype(jnp.float32)
    h = (rg_lru(xc, wa_f, ba_f, wx_f, bx_f, lam_f, False)
         + rg_lru(xc, wa_b, ba_b, wx_b, bx_b, lam_b, True))
    y = h.astype(x.dtype) * jax.nn.gelu(gate, approximate=True)
    return y @ w_out


def grouped_moe(x, router_w, router_bias, w1, w3, w2):
    f32 = jnp.float32
    bsz, seq, d = x.shape
    t = x.reshape(bsz * seq, d)
    scores = jax.nn.sigmoid(t.astype(f32) @ router_w.astype(f32))
    biased = scores + router_bias.astype(f32)
    group_score = lax.top_k(biased.reshape(-1, N_GROUPS, EXPERTS_PER_GROUP), GROUP_SCORE_K)[0].sum(-1)
    group_mask = jnp.argmax(group_score, axis=-1)[:, None] == jnp.arange(N_GROUPS)[None, :]
    expert_mask = jnp.repeat(group_mask, EXPERTS_PER_GROUP, axis=-1)
    _, idx = lax.top_k(jnp.where(expert_mask, biased, -jnp.inf), TOP_K)
    w = jnp.take_along_axis(scores, idx, axis=-1)
    w = w / jnp.sum(w, axis=-1, keepdims=True)
    gates = jnp.einsum('tk,tke->te', w, jax.nn.one_hot(idx, N_EXPERTS, dtype=f32)).astype(x.dtype)
    y = jnp.zeros_like(t)
    for e in range(N_EXPERTS):
        h = jax.nn.silu(t @ w1[e]) * (t @ w3[e])
        y = y + gates[:, e:e + 1] * (h @ w2[e])
    return y.reshape(bsz, seq, d)


def setup_inputs(seed: int = 0) -> dict:
    key = jax.random.key(seed)
    ks = iter(jax.random.split(key, 48))
    f32 = jnp.float32
    D = D_MODEL

    def nrm(shape, scale):
        return jax.random.normal(next(ks), shape, f32) * scale

    def gain(n):
        return 1.0 + nrm((n,), 0.01)

    def lru_lambda():
        u = jax.random.uniform(next(ks), (LRU_WIDTH,), f32, minval=0.9, maxval=0.999)
        a = u ** (1.0 / LRU_C)
        return jnp.log(a) - jnp.log1p(-a)

    inp = {}
    inp['x'] = nrm((BATCH, SEQ, D), 1.0)
    inp['rel_bias'] = nrm((REL_BUCKETS, SWA_HEADS), 0.2)
    inp['router_w'] = nrm((D, N_EXPERTS), D ** -0.5)
    inp['router_bias'] = nrm((N_EXPERTS,), 0.01)
    inp['l0_w_in'] = nrm((D, IN0_COLS), D ** -0.5)
    inp['l0_q_norm'] = gain(MLA_Q_RANK)
    inp['l0_w_uq'] = nrm((MLA_Q_RANK, MLA_HEADS * MLA_QK_DIM), MLA_Q_RANK ** -0.5)
    inp['l0_kv_norm'] = gain(MLA_KV_RANK)
    inp['l0_w_ukv'] = nrm((MLA_KV_RANK, MLA_HEADS * (MLA_NOPE_DIM + MLA_V_DIM)), MLA_KV_RANK ** -0.5)
    inp['l0_sinks'] = nrm((SWA_HEADS,), 0.5)
    inp['l0_w_out'] = nrm((MIX0_WIDTH, D), MIX0_WIDTH ** -0.5 * BETA)
    inp['l0_ln1_g'] = gain(D)
    inp['l0_ln1_b'] = nrm((D,), 0.01)
    inp['l0_w1'] = nrm((N_EXPERTS, D, D_EXPERT), D ** -0.5)
    inp['l0_w3'] = nrm((N_EXPERTS, D, D_EXPERT), D ** -0.5)
    inp['l0_w2'] = nrm((N_EXPERTS, D_EXPERT, D), D_EXPERT ** -0.5 * BETA)
    inp['l0_ln2_g'] = gain(D)
    inp['l0_ln2_b'] = nrm((D,), 0.01)
    inp['l1_w_in'] = nrm((D, 2 * LRU_WIDTH), D ** -0.5)
    inp['l1_conv_w'] = nrm((CONV_WIDTH, LRU_WIDTH), CONV_WIDTH ** -0.5)
    inp['l1_conv_b'] = nrm((LRU_WIDTH,), 0.01)
    inp['l1_wa_f'] = nrm((LRU_BLOCKS, LRU_BW, LRU_BW), LRU_BW ** -0.5)
    inp['l1_ba_f'] = nrm((LRU_BLOCKS, LRU_BW), 0.01)
    inp['l1_wx_f'] = nrm((LRU_BLOCKS, LRU_BW, LRU_BW), LRU_BW ** -0.5)
    inp['l1_bx_f'] = nrm((LRU_BLOCKS, LRU_BW), 0.01)
    inp['l1_lam_f'] = lru_lambda()
    inp['l1_wa_b'] = nrm((LRU_BLOCKS, LRU_BW, LRU_BW), LRU_BW ** -0.5)
    inp['l1_ba_b'] = nrm((LRU_BLOCKS, LRU_BW), 0.01)
    inp['l1_wx_b'] = nrm((LRU_BLOCKS, LRU_BW, LRU_BW), LRU_BW ** -0.5)
    inp['l1_bx_b'] = nrm((LRU_BLOCKS, LRU_BW), 0.01)
    inp['l1_lam_b'] = lru_lambda()
    inp['l1_w_out'] = nrm((LRU_WIDTH, D), LRU_WIDTH ** -0.5 * BETA)
    inp['l1_ln1_g'] = gain(D)
    inp['l1_ln1_b'] = nrm((D,), 0.01)
    inp['l1_w1'] = nrm((N_EXPERTS, D, D_EXPERT), D ** -0.5)
    inp['l1_w3'] = nrm((N_EXPERTS, D, D_EXPERT), D ** -0.5)
    inp['l1_w2'] = nrm((N_EXPERTS, D_EXPERT, D), D_EXPERT ** -0.5 * BETA)
    inp['l1_ln2_g'] = gain(D)
    inp['l1_ln2_b'] = nrm((D,), 0.01)
    return inp


def reference(x, rel_bias, router_w, router_bias,
              l0_w_in, l0_q_norm, l0_w_uq, l0_kv_norm, l0_w_ukv, l0_sinks, l0_w_out,
              l0_ln1_g, l0_ln1_b, l0_w1, l0_w3, l0_w2, l0_ln2_g, l0_ln2_b,
              l1_w_in, l1_conv_w, l1_conv_b, l1_wa_f, l1_ba_f, l1_wx_f, l1_bx_f, l1_lam_f,
              l1_wa_b, l1_ba_b, l1_wx_b, l1_bx_b, l1_lam_b, l1_w_out,
              l1_ln1_g, l1_ln1_b, l1_w1, l1_w3, l1_w2, l1_ln2_g, l1_ln2_b):
    cos, sin = rotary_tables(x.shape[1], x.dtype)
    mixer_params = [
        (l0_w_in, l0_q_norm, l0_w_uq, l0_kv_norm, l0_w_ukv, l0_sinks, l0_w_out),
        (l1_w_in, l1_conv_w, l1_conv_b, l1_wa_f, l1_ba_f, l1_wx_f, l1_bx_f, l1_lam_f,
         l1_wa_b, l1_ba_b, l1_wx_b, l1_bx_b, l1_lam_b, l1_w_out),
    ]
    norm1 = [(l0_ln1_g, l0_ln1_b), (l1_ln1_g, l1_ln1_b)]
    experts = [(l0_w1, l0_w3, l0_w2), (l1_w1, l1_w3, l1_w2)]
    norm2 = [(l0_ln2_g, l0_ln2_b), (l1_ln2_g, l1_ln2_b)]
    h = x
    for layer in range(DEPTH):
        if layer % 2 == 0:
            mixed = mixer_attention_pair(h, *mixer_params[layer], rel_bias, cos, sin)
        else:
            mixed = mixer_rglru(h, *mixer_params[layer])
        h = layer_norm(ALPHA * h + mixed, *norm1[layer])
        h = layer_norm(ALPHA * h + grouped_moe(h, router_w, router_bias, *experts[layer]), *norm2[layer])
    return h
```

```python
import math
from contextlib import ExitStack
import numpy as np
import concourse.bass as bass
import concourse.mybir as mybir
from concourse.bass_utils import run_bass_kernel_spmd

F32 = mybir.dt.float32
BF16 = mybir.dt.bfloat16
AF = mybir.ActivationFunctionType
ALU = mybir.AluOpType
AX = mybir.AxisListType

NCORES = 8
D = 1024
SEQ = 8192
TOK = 2048
NT = TOK // 128
ALPHA = (2.0 * 2) ** 0.25
LN_EPS = 1e-5
RMS_EPS = 1e-6
NEG = -30000.0
BIG = 1.0e4


class Buf:
    def __init__(self):
        self.w = None
        self.r = {}


class DSem:
    def __init__(self, kb, name):
        self.sem = kb.es.enter_context(kb.nc.semaphore(name))
        self.cnt = 0


class Eng:
    def __init__(self, kb, eng, name):
        self.kb = kb
        self.e = eng
        self.sem = kb.es.enter_context(kb.nc.semaphore("s_" + name))
        self.cnt = 0
        self.waited = {}
        self.name = name

    def _need(self, reads, writes):
        need = {}
        for b in reads:
            if b.w is not None:
                need[b.w[0]] = max(need.get(b.w[0], 0), b.w[1])
        for b in writes:
            if b.w is not None and b.w[0] is not self:
                need[b.w[0]] = max(need.get(b.w[0], 0), b.w[1])
            for s, c in b.r.items():
                if s is not self:
                    need[s] = max(need.get(s, 0), c)
        return need

    def _pre(self, reads, writes):
        for s, c in self._need(reads, writes).items():
            if s is self and self.name == "pe":
                continue
            if isinstance(s, DSem):
                c = s.cnt
            assert c <= s.cnt, f"unfulfilled promise {getattr(s, 'name', 'dma')} {c}>{s.cnt}"
            if self.waited.get(s, 0) < c:
                self.e.wait_ge(s.sem, c)
                self.waited[s] = c

    def _post(self, ins, reads, writes, sig):
        if sig:
            self.cnt += 1
            ins.then_inc(self.sem, 1)
            tk = self.cnt
        else:
            tk = self.cnt + 1
        for b in reads:
            b.r[self] = tk
        for b in writes:
            b.w = (self, tk)
            b.r = {}

    def op(self, name, *args, reads=(), writes=(), sig=True, **kw):
        self._pre(reads, writes)
        ins = getattr(self.e, name)(*args, **kw)
        self._post(ins, reads, writes, sig)
        return ins

    def dma(self, out, in_, ds, reads=(), writes=(), **kw):
        self._pre(reads, writes)
        ins = self.e.dma_start(out=out, in_=in_, **kw)
        ds.cnt += 16
        ins.then_inc(ds.sem, 16)
        for b in reads:
            b.r[ds] = ds.cnt
        for b in writes:
            b.w = (ds, ds.cnt)
            b.r = {}

    def wait_all(self, sems):
        for s in sems:
            if s is self:
                continue
            if self.waited.get(s, 0) < s.cnt:
                self.e.wait_ge(s.sem, s.cnt)
                self.waited[s] = s.cnt


class Tile(Buf):
    def __init__(self, kb, es, name, shape, dtype, psum=False):
        super().__init__()
        f = kb.nc.psum_tensor if psum else kb.nc.sbuf_tensor
        self.t = es.enter_context(f(name, shape, dtype))
        self.ds = None

    def __getitem__(self, idx):
        return self.t[idx]


class KB:
    def __init__(self, nc):
        self.nc = nc
        self.es = ExitStack()
        self.pe = Eng(self, nc.tensor, "pe")
        self.act = Eng(self, nc.scalar, "act")
        self.dve = Eng(self, nc.vector, "dve")
        self.pool = Eng(self, nc.gpsimd, "pool")
        self.sp = Eng(self, nc.sync, "sp")
        self.engs = [self.pe, self.act, self.dve, self.pool, self.sp]
        self.dsems = []
        self.ps = [Tile(self, self.es, f"ps{i}", [128, 512], F32, psum=True) for i in range(8)]
        self.psi = 0
        self.uid = 0

    def dsem(self, name):
        d = DSem(self, name)
        self.dsems.append(d)
        return d

    def bank(self):
        b = self.ps[self.psi % 8]
        self.psi += 1
        return b

    def tile(self, es, name, shape, dtype):
        self.uid += 1
        return Tile(self, es, f"{name}_{self.uid}", shape, dtype)

    def barrier(self):
        allsems = self.engs + self.dsems
        for e in self.engs:
            e.wait_all(allsems)


def dram_in(nc, name, shape, dt=F32):
    return nc.dram_tensor(name, list(shape), dt, kind="ExternalInput").ap()


def emit_ln(kb, es, acc, accb, lnp, ln_idx, want_router, rw_t, sc_t, hT, out_dram, res_scale, d_ld):
    nc = kb.nc
    pe, act, dve, pool, sp = kb.pe, kb.act, kb.dve, kb.pool, kb.sp
    ln_tok, ln_fm, ident = lnp
    g_t = kb.tile(es, "ln_g", [128, 1024], F32)
    b_t = kb.tile(es, "ln_b", [128, 1024], F32)
    fm = kb.tile(es, "ln_fm", [128, 16], F32)
    sp.dma(g_t[:, :], ln_tok[ln_idx, 0, :, :], d_ld, writes=[g_t])
    sp.dma(b_t[:, :], ln_tok[ln_idx, 1, :, :], d_ld, writes=[b_t])
    sp.dma(fm[:, :], ln_fm[ln_idx, :, :], d_ld, writes=[fm])
    if res_scale != 1.0:
        pool.op("tensor_scalar", g_t[:, :], g_t[:, :], float(res_scale), None, op0=ALU.mult, reads=[g_t], writes=[g_t])
        pool.op("tensor_scalar", b_t[:, :], b_t[:, :], float(res_scale), None, op0=ALU.mult, reads=[b_t], writes=[b_t])
    xn_r = [kb.tile(es, "xn", [128, 1024], F32) for _ in range(2)]
    h32_r = [kb.tile(es, "h32", [128, 8, 128], F32) for _ in range(2)]
    st_r = [kb.tile(es, "lnst", [128, 16], F32) for _ in range(2)]
    d_out = kb.dsem("d_lnout") if out_dram is not None else None
    for tt in range(NT):
        xn, h32, st = xn_r[tt % 2], h32_r[tt % 2], st_r[tt % 2]
        a = acc[:, tt, :]
        act.op("activation", xn[:, :], a, AF.Square, reads=[accb[tt]], writes=[xn])
        dve.op("tensor_reduce", st[:, 0:1], a, axis=AX.X, op=ALU.add, reads=[accb[tt]], writes=[st])
        dve.op("tensor_reduce", st[:, 1:2], xn[:, :], axis=AX.X, op=ALU.add, reads=[xn], writes=[st])
        dve.op("tensor_scalar", st[:, 2:3], st[:, 0:1], 1.0 / 1024, None, op0=ALU.mult, reads=[st], writes=[st])
        dve.op("tensor_tensor", st[:, 3:4], st[:, 2:3], st[:, 2:3], op=ALU.mult, reads=[st], writes=[st])
        dve.op("scalar_tensor_tensor", st[:, 4:5], st[:, 1:2], 1.0 / 1024, st[:, 3:4], op0=ALU.mult, op1=ALU.subtract, reads=[st], writes=[st])
        act.op("activation", st[:, 5:6], st[:, 4:5], AF.Sqrt, bias=LN_EPS, scale=1.0, reads=[st], writes=[st])
        dve.op("reciprocal", st[:, 6:7], st[:, 5:6], reads=[st], writes=[st])
        dve.op("tensor_scalar", xn[:, :], a, st[:, 2:3], st[:, 6:7], op0=ALU.subtract, op1=ALU.mult,
               reads=[accb[tt], st], writes=[xn])
        if hT is not None:
            for half in range(2):
                ps = kb.bank()
                for j in range(4):
                    k = half * 4 + j
                    pe.op("transpose", ps[:, j * 128:(j + 1) * 128], xn[:, k * 128:(k + 1) * 128], ident[:, :],
                          reads=[xn, ident], writes=[ps], sig=(j == 3))
                for j in range(4):
                    k = half * 4 + j
                    act.op("activation", h32[:, k, :], ps[:, j * 128:(j + 1) * 128], AF.Identity,
                           scale=fm[:, k:k + 1], bias=fm[:, 8 + k:9 + k], reads=[ps, fm], writes=[h32])
            dve.op("tensor_copy", hT[:, :, tt * 128:(tt + 1) * 128], h32[:, :, :], reads=[h32], writes=[hT.tb[tt]])
            if want_router:
                ps = kb.bank()
                for k in range(8):
                    pe.op("matmul", ps[:, 0:16], lhsT=h32[:, k, :], rhs=rw_t[:, k, :], start=(k == 0), stop=(k == 7),
                          reads=[h32, rw_t], writes=[ps], sig=(k == 7))
                act.op("activation", sc_t[:, tt, :], ps[:, 0:16], AF.Sigmoid, reads=[ps], writes=[sc_t])
        pool.op("tensor_tensor", xn[:, :], xn[:, :], g_t[:, :], op=ALU.mult, reads=[xn, g_t], writes=[xn])
        pool.op("tensor_tensor", a, xn[:, :], b_t[:, :], op=ALU.add, reads=[xn, b_t], writes=[accb[tt]])
        if out_dram is not None:
            sp.dma(out_dram[tt * 128:(tt + 1) * 128, :], a, d_out, reads=[accb[tt]])
    return d_out


def emit_moe(kb, es, acc, accb, hT, sc_t, rb_t, w1, w3, w2, d_ld):
    nc = kb.nc
    pe, act, dve, pool, sp = kb.pe, kb.act, kb.dve, kb.pool, kb.sp
    V = lambda t: t[:, :].rearrange("p (a g j) -> p a g j", a=NT, g=4)
    V3 = lambda t: t[:, :].rearrange("p (a e) -> p a e", a=NT)
    bi = kb.tile(es, "r_bi", [128, 256], F32)
    t0 = kb.tile(es, "r_t0", [128, 64], F32)
    gs = kb.tile(es, "r_gs", [128, 64], F32)
    gm = kb.tile(es, "r_gm", [128, 16], F32)
    pen = kb.tile(es, "r_pen", [128, 64], F32)
    mk = kb.tile(es, "r_mk", [128, 256], F32)
    m1 = kb.tile(es, "r_m1", [128, 256], F32)
    m2 = kb.tile(es, "r_m2", [128, 256], F32)
    gates = kb.tile(es, "r_gates", [128, 256], F32)
    scv = sc_t[:, :, :].rearrange("p a e -> p (a e)")
    dve.op("tensor_tensor", bi[:, :], scv, rb_t[:, :], op=ALU.add, reads=[sc_t, rb_t], writes=[bi])
    b4 = V(bi)
    g3 = gs[:, :].rearrange("p (a g) -> p a g", a=NT)
    t3 = t0[:, :].rearrange("p (a g) -> p a g", a=NT)
    first = True
    for j1 in range(4):
        for j2 in range(j1 + 1, 4):
            if first:
                dve.op("tensor_tensor", g3, b4[:, :, :, j1], b4[:, :, :, j2], op=ALU.add, reads=[bi], writes=[gs])
                first = False
            else:
                dve.op("tensor_tensor", t3, b4[:, :, :, j1], b4[:, :, :, j2], op=ALU.add, reads=[bi], writes=[t0])
                dve.op("tensor_tensor", g3, g3, t3, op=ALU.max, reads=[gs, t0], writes=[gs])
    dve.op("tensor_reduce", gm[:, :], g3, axis=AX.X, op=ALU.max, reads=[gs], writes=[gm])
    p3 = pen[:, :].rearrange("p (a g) -> p a g", a=NT)
    for g in range(4):
        dve.op("tensor_tensor", p3[:, :, g], g3[:, :, g], gm[:, :], op=ALU.is_equal, reads=[gs, gm], writes=[pen])
    dve.op("tensor_scalar", pen[:, :], pen[:, :], 1.0, BIG, op0=ALU.subtract, op1=ALU.mult, reads=[pen], writes=[pen])
    mk4 = V(mk)
    for j in range(4):
        dve.op("tensor_tensor", mk4[:, :, :, j], b4[:, :, :, j], p3, op=ALU.add, reads=[bi, pen], writes=[mk])
    mk3, m13, m23 = V3(mk), V3(m1), V3(m2)
    dve.op("tensor_reduce", gm[:, :], mk3, axis=AX.X, op=ALU.max, reads=[mk], writes=[gm])
    for e in range(16):
        dve.op("tensor_tensor", m13[:, :, e], mk3[:, :, e], gm[:, :], op=ALU.is_equal, reads=[mk, gm], writes=[m1])
    dve.op("scalar_tensor_tensor", m2[:, :], m1[:, :], -BIG, mk[:, :], op0=ALU.mult, op1=ALU.add, reads=[m1, mk], writes=[m2])
    dve.op("tensor_reduce", gm[:, :], m23, axis=AX.X, op=ALU.max, reads=[m2], writes=[gm])
    for e in range(16):
        dve.op("tensor_tensor", mk3[:, :, e], m23[:, :, e], gm[:, :], op=ALU.is_equal, reads=[m2, gm], writes=[mk])
    dve.op("tensor_tensor", m1[:, :], m1[:, :], mk[:, :], op=ALU.add, reads=[m1, mk], writes=[m1])
    dve.op("tensor_tensor", m1[:, :], m1[:, :], scv, op=ALU.mult, reads=[m1, sc_t], writes=[m1])
    dve.op("tensor_reduce", gm[:, :], m13, axis=AX.X, op=ALU.add, reads=[m1], writes=[gm])
    dve.op("reciprocal", gm[:, :], gm[:, :], reads=[gm], writes=[gm])
    g3g = V3(gates)
    for e in range(16):
        dve.op("tensor_tensor", g3g[:, :, e], m13[:, :, e], gm[:, :], op=ALU.mult, reads=[m1, gm], writes=[gates])
    slots = []
    for s in range(2):
        sl = dict(w1=kb.tile(es, "w1s", [128, 8, 512], BF16), w3=kb.tile(es, "w3s", [128, 8, 512], BF16),
                  w2=kb.tile(es, "w2s", [128, 4, 1024], BF16), ds=kb.dsem(f"d_moe{s}"))
        slots.append(sl)
    gT_r = [kb.tile(es, "gT", [128, 4, 512], BF16) for _ in range(2)]
    sil_r = [kb.tile(es, "sil", [128, 512], F32) for _ in range(2)]

    def load(e):
        sl = slots[e % 2]
        pool.dma(sl["w1"][:, :, :], w1[e].rearrange("(k p) n -> p k n", p=128), sl["ds"], writes=[sl["w1"]])
        pool.dma(sl["w3"][:, :, :], w3[e].rearrange("(k p) n -> p k n", p=128), sl["ds"], writes=[sl["w3"]])
        pool.dma(sl["w2"][:, :, :], w2[e].rearrange("(k p) n -> p k n", p=128), sl["ds"], writes=[sl["w2"]])

    load(0)
    it = 0
    for e in range(16):
        if e + 1 < 16:
            load(e + 1)
        sl = slots[e % 2]
        for nt in range(4):
            gT = gT_r[it % 2]
            for m in range(4):
                pa, pb = kb.bank(), kb.bank()
                for k in range(8):
                    pe.op("matmul", pa[:, :], lhsT=sl["w1"][:, k, m * 128:(m + 1) * 128], rhs=hT[:, k, nt * 512:(nt + 1) * 512],
                          start=(k == 0), stop=(k == 7), reads=[sl["w1"]] + hT.tb[nt * 4:nt * 4 + 4], writes=[pa], sig=(k == 7))
                for k in range(8):
                    pe.op("matmul", pb[:, :], lhsT=sl["w3"][:, k, m * 128:(m + 1) * 128], rhs=hT[:, k, nt * 512:(nt + 1) * 512],
                          start=(k == 0), stop=(k == 7), reads=[sl["w3"]] + hT.tb[nt * 4:nt * 4 + 4], writes=[pb], sig=(k == 7))
                sil = sil_r[(it * 4 + m) % 2]
                act.op("activation", sil[:, :], pa[:, :], AF.Silu, reads=[pa], writes=[sil])
                dve.op("tensor_tensor", gT[:, m, :], pb[:, :], sil[:, :], op=ALU.mult, reads=[pb, sil], writes=[gT])
            for tq in range(4):
                tt = nt * 4 + tq
                for half in range(2):
                    py = kb.bank()
                    for m in range(4):
                        pe.op("matmul", py[:, :], lhsT=gT[:, m, tq * 128:(tq + 1) * 128], rhs=sl["w2"][:, m, half * 512:(half + 1) * 512],
                              start=(m == 0), stop=(m == 3), reads=[gT, sl["w2"]], writes=[py], sig=(m == 3))
                    aa = acc[:, tt, half * 512:(half + 1) * 512]
                    dve.op("scalar_tensor_tensor", aa, py[:, :], g3g[:, tt, e:e + 1], aa, op0=ALU.mult, op1=ALU.add,
                           reads=[py, gates, accb[tt]], writes=[accb[tt]])
            it += 1


def emit_l0_mixer(kb, I, catT):
    nc = kb.nc
    pe, act, dve, pool, sp = kb.pe, kb.act, kb.dve, kb.pool, kb.sp
    d_ld = kb.dsem("d_l0ld")
    esP = ExitStack()
    kvnT = kb.tile(esP, "kvnT", [128, SEQ], BF16)
    kpeT = kb.tile(esP, "kpeT", [96, SEQ], BF16)
    qnT = kb.tile(esP, "qnT", [128, 2, TOK], BF16)
    ones = kb.tile(esP, "ones", [128, 128], BF16)
    dve.op("memset", ones[:, :], 1.0, writes=[ones])
    wukv = kb.tile(esP, "wukv", [128, 1024], BF16)
    wuq = kb.tile(esP, "wuq", [128, 2, 768], BF16)
    wuqr = kb.tile(esP, "wuqr", [128, 2, 8, 96], BF16)

    esBD = ExitStack()
    qbT = kb.tile(esBD, "qbT", [128, 4, TOK], BF16)
    kbT = kb.tile(esBD, "kbT", [128, 18 * 128], BF16)
    vaw = kb.tile(esBD, "vaw", [128, 18, 2, 2, 128], BF16)
    es = ExitStack()
    esA = ExitStack()
    win = kb.tile(es, "win0", [128, 8, 1184], BF16)
    pool.dma(win[:, :, :], I["l0_w_in"].rearrange("(k p) n -> p k n", p=128), d_ld, writes=[win])
    xr_ = [kb.tile(es, "xs", [128, 8, 512], BF16) for _ in range(2)]
    xds = [kb.dsem(f"d_xs{i}") for i in range(2)]
    sq_ = [kb.tile(es, "sq", [128, 2, 512], BF16) for _ in range(2)]
    rs_ = [kb.tile(es, "rs", [128, 512], F32) for _ in range(2)]
    wkr = kb.tile(esA, "wkr", [128, 8, 96], BF16)
    wkrr = kb.tile(esA, "wkrr", [128, 8, 96], BF16)
    stg = kb.tile(esA, "stg", [128, 2, 1024], F32)
    vec = kb.tile(esA, "vec", [128, 16], F32)
    sp.dma(vec[:, 0:2], I["qn_g"][:, :], d_ld, writes=[vec])
    sp.dma(vec[:, 2:3], I["kvn_g"][:, :], d_ld, writes=[vec])
    sp.dma(stg[:, 0, :], I["l0_w_ukv"][:, :], d_ld, writes=[stg])
    dve.op("tensor_scalar", wukv[:, :], stg[:, 0, :], vec[:, 2:3], None, op0=ALU.mult, reads=[stg, vec], writes=[wukv])
    sp.dma(stg[:, :, 0:768], I["l0_w_uq"].rearrange("(k p) n -> p k n", p=128), d_ld, writes=[stg])
    for c in range(2):
        dve.op("tensor_scalar", wuq[:, c, :], stg[:, c, 0:768], vec[:, c:c + 1], None, op0=ALU.mult, reads=[stg, vec], writes=[wuq])
    dve.op("memset", wuqr[:, :, :, :], 0.0, writes=[wuqr])
    wq4 = wuq[:, :, :].rearrange("p c (h d) -> p c h d", h=8)
    for c in range(2):
        dve.op("tensor_scalar", wuqr[:, c, :, 64:80], wq4[:, c, :, 80:96], -1.0, None, op0=ALU.mult, reads=[wuq], writes=[wuqr])
        dve.op("tensor_copy", wuqr[:, c, :, 80:96], wq4[:, c, :, 64:80], reads=[wuq], writes=[wuqr])
    dve.op("memset", wkr[:, :, :], 0.0, writes=[wkr])
    dve.op("memset", wkrr[:, :, :], 0.0, writes=[wkrr])
    dve.op("tensor_copy", wkr[:, :, 64:96], win[:, :, 384:416], reads=[win], writes=[wkr])
    dve.op("tensor_scalar", wkrr[:, :, 64:80], win[:, :, 400:416], -1.0, None, op0=ALU.mult, reads=[win], writes=[wkrr])
    dve.op("tensor_copy", wkrr[:, :, 80:96], win[:, :, 384:400], reads=[win], writes=[wkrr])

    cs_ = [kb.tile(esA, "cs", [96, 2, 512], F32) for _ in range(2)]
    cds = [kb.dsem(f"d_cs{i}") for i in range(2)]
    t12_ = [kb.tile(esA, "t12", [96, 2, 512], F32) for _ in range(2)]

    xTf = I["xT_full"].rearrange("(k p) t -> p k t", p=128)
    for t in range(SEQ // 512):
        xs, cs, sq, rs, t12 = xr_[t % 2], cs_[t % 2], sq_[t % 2], rs_[t % 2], t12_[t % 2]
        sl = slice(t * 512, (t + 1) * 512)
        pool.dma(xs[:, :, :], xTf[:, :, sl], xds[t % 2], writes=[xs], max_dma_last_dim=2048)
        sp.dma(cs[64:96, 0, :], I["cs_full"][0, :, sl], cds[t % 2], writes=[cs])
        sp.dma(cs[64:96, 1, :], I["cs_full"][1, :, sl], cds[t % 2], writes=[cs])
        pkv, pA, pB, pss = kb.bank(), kb.bank(), kb.bank(), kb.bank()
        for k in range(8):
            pe.op("matmul", pkv[:, :], lhsT=win[:, k, 256:384], rhs=xs[:, k, :], start=(k == 0), stop=(k == 7),
                  reads=[win, xs], writes=[pkv], sig=(k == 7))
        for k in range(8):
            pe.op("matmul", pA[0:96, :], lhsT=wkr[:, k, :], rhs=xs[:, k, :], start=(k == 0), stop=(k == 7),
                  reads=[wkr, xs], writes=[pA], sig=(k == 7))
        for k in range(8):
            pe.op("matmul", pB[0:96, :], lhsT=wkrr[:, k, :], rhs=xs[:, k, :], start=(k == 0), stop=(k == 7),
                  reads=[wkrr, xs], writes=[pB], sig=(k == 7))
        act.op("activation", sq[:, 0, :], pkv[:, :], AF.Square, reads=[pkv], writes=[sq])
        pe.op("matmul", pss[:, :], lhsT=ones[:, :], rhs=sq[:, 0, :], start=True, stop=True, reads=[ones, sq], writes=[pss])
        act.op("activation", rs[:, :], pss[:, :], AF.Sqrt, scale=1.0 / 128, bias=RMS_EPS, reads=[pss], writes=[rs])
        dve.op("reciprocal", rs[:, :], rs[:, :], reads=[rs], writes=[rs])
        dve.op("tensor_tensor", kvnT[:, sl], pkv[:, :], rs[:, :], op=ALU.mult, reads=[pkv, rs], writes=[kvnT])
        dve.op("tensor_tensor", t12[64:96, 0, :], pA[64:96, :], cs[64:96, 0, :], op=ALU.mult, reads=[pA, cs], writes=[t12])
        dve.op("tensor_tensor", t12[64:96, 1, :], pB[64:96, :], cs[64:96, 1, :], op=ALU.mult, reads=[pB, cs], writes=[t12])
        dve.op("tensor_tensor", kpeT[64:96, sl], t12[64:96, 0, :], t12[64:96, 1, :], op=ALU.add, reads=[t12], writes=[kpeT])
    kb.barrier()
    esA.close()

    dve.op("memset", vaw[:, :, :, :, :].rearrange("p a g v d -> p (a g v d)"), 1.0, writes=[vaw])
    xTo = I["xT_own"].rearrange("(k p) t -> p k t", p=128)
    xTh = I["xT_halo"].rearrange("(k p) t -> p k t", p=128)
    tno = 0
    for t in range(5):
        xs, sq, rs = xr_[tno % 2], sq_[tno % 2], rs_[tno % 2]
        if t < 4:
            W = 512
            pool.dma(xs[:, :, :], xTo[:, :, t * 512:(t + 1) * 512], xds[tno % 2], writes=[xs], max_dma_last_dim=2048)
            blk0 = 1 + t * 4
            ksl = slice(128 + t * 512, 128 + (t + 1) * 512)
        else:
            W = 256
            pool.dma(xs[:, :, 0:256], xTh[:, :, :], xds[tno % 2], writes=[xs], max_dma_last_dim=2048)
        tno += 1
        if t < 4:
            sl = slice(t * 512, (t + 1) * 512)
            p0, p1, pss = kb.bank(), kb.bank(), kb.bank()
            for c, pq in enumerate((p0, p1)):
                for k in range(8):
                    pe.op("matmul", pq[:, :], lhsT=win[:, k, c * 128:(c + 1) * 128], rhs=xs[:, k, :], start=(k == 0), stop=(k == 7),
                          reads=[win, xs], writes=[pq], sig=(k == 7))
                act.op("activation", sq[:, c, :], pq[:, :], AF.Square, reads=[pq], writes=[sq])
            for c in range(2):
                pe.op("matmul", pss[:, :], lhsT=ones[:, :], rhs=sq[:, c, :], start=(c == 0), stop=(c == 1), reads=[ones, sq], writes=[pss], sig=(c == 1))
            act.op("activation", rs[:, :], pss[:, :], AF.Sqrt, scale=1.0 / 256, bias=RMS_EPS, reads=[pss], writes=[rs])
            dve.op("reciprocal", rs[:, :], rs[:, :], reads=[rs], writes=[rs])
            for c, pq in enumerate((p0, p1)):
                dve.op("tensor_tensor", qnT[:, c, sl], pq[:, :], rs[:, :], op=ALU.mult, reads=[pq, rs], writes=[qnT])
            for c in range(4):
                pq = kb.bank()
                for k in range(8):
                    pe.op("matmul", pq[:, :], lhsT=win[:, k, 416 + c * 128:416 + (c + 1) * 128], rhs=xs[:, k, :], start=(k == 0), stop=(k == 7),
                          reads=[win, xs], writes=[pq], sig=(k == 7))
                act.op("copy", qbT[:, c, sl], pq[:, :], reads=[pq], writes=[qbT])
        pk = kb.bank()
        for k in range(8):
            pe.op("matmul", pk[:, 0:W], lhsT=win[:, k, 928:1056], rhs=xs[:, k, 0:W], start=(k == 0), stop=(k == 7),
                  reads=[win, xs], writes=[pk], sig=(k == 7))
        if t < 4:
            act.op("copy", kbT[:, ksl], pk[:, :], reads=[pk], writes=[kbT])
        else:
            act.op("copy", kbT[:, 0:128], pk[:, 0:128], reads=[pk], writes=[kbT])
            act.op("copy", kbT[:, 17 * 128:18 * 128], pk[:, 128:256], reads=[pk], writes=[kbT])
        pv = kb.bank()
        nb = W // 128
        for s in range(nb):
            for k in range(8):
                pe.op("matmul", pv[:, s * 128:(s + 1) * 128], lhsT=xs[:, k, s * 128:(s + 1) * 128], rhs=win[:, k, 1056:1184],
                      start=(k == 0), stop=(k == 7), reads=[win, xs], writes=[pv], sig=(k == 7 and s == nb - 1))
        for s in range(nb):
            blk = (blk0 + s) if t < 4 else (0 if s == 0 else 17)
            for g in range(2):
                dve.op("tensor_copy", vaw[:, blk, g, 0, 0:64], pv[:, s * 128 + g * 64:s * 128 + (g + 1) * 64], reads=[pv], writes=[vaw])
                dve.op("tensor_copy", vaw[:, blk, g, 1, 64:128], pv[:, s * 128 + g * 64:s * 128 + (g + 1) * 64], reads=[pv], writes=[vaw])

    kb.barrier()
    es.close()
    es = ExitStack()
    bT = kb.tile(es, "swab", [128, 3, 8, 128], F32)
    mT = kb.tile(es, "swam", [128, 3, 8, 128], F32)
    sp.dma(bT[:, :, :, :], I["swa_bias"][:, :, :, :], d_ld, writes=[bT])
    sp.dma(mT[:, :, :, :], I["swa_mask"][:, :, :, :], d_ld, writes=[mT])
    fl = lambda t_: t_[:, :, :, :].rearrange("p a h q -> p (a h q)")
    dve.op("tensor_tensor", fl(bT), fl(bT), fl(mT), op=ALU.add, reads=[bT, mT], writes=[bT])
    edge = kb.tile(es, "edge", [128, 2], F32)
    sp.dma(edge[:, :], I["edge"][:, :], d_ld, writes=[edge])
    snk = kb.tile(es, "snk", [128, 8], F32)
    sp.dma(snk[:, :], I["sinks_b"][:, :], d_ld, writes=[snk])
    act.op("activation", snk[:, :], snk[:, :], AF.Exp, reads=[snk], writes=[snk])
    sinkE = kb.tile(es, "sinkE", [128, 8, 128], F32)
    dve.op("memset", sinkE[:, :, :], 0.0, writes=[sinkE])
    for h in range(8):
        dve.op("tensor_scalar", sinkE[:, h, :], sinkE[:, h, :], snk[:, h:h + 1], None, op0=ALU.add, reads=[sinkE, snk], writes=[sinkE])
    sb_ = [kb.tile(es, "swsb", [128, 512], F32) for _ in range(2)]
    pw_ = [kb.tile(es, "swp", [128, 4, 128], BF16) for _ in range(3)]
    den_ = [kb.tile(es, "swden", [128, 2, 256], F32) for _ in range(2)]
    sscale = 64 ** -0.5
    it = 0
    for qb in range(NT):
        qs = slice(qb * 128, (qb + 1) * 128)
        for g in range(2):
            gp = slice(g * 64, (g + 1) * 64)
            pO = [kb.bank(), kb.bank()]
            for rb in range(3):
                blk = qb + rb
                pS = kb.bank()
                sb, pw = sb_[it % 2], pw_[it % 3]
                it += 1
                pe.op("matmul", pS[:, :].rearrange("p (c q) -> p c q", c=4), lhsT=kbT[gp, blk * 128:(blk + 1) * 128], rhs=qbT[gp, :, qs],
                      start=True, stop=True, reads=[kbT, qbT], writes=[pS])
                dve.op("scalar_tensor_tensor", sb[:, :].rearrange("p (c q) -> p c q", c=4), pS[:, :].rearrange("p (c q) -> p c q", c=4),
                       sscale, bT[:, rb, g * 4:(g + 1) * 4, :], op0=ALU.mult, op1=ALU.add, reads=[pS, bT], writes=[sb])
                kw = {}
                if qb == 0 and rb == 0:
                    kw = dict(bias=edge[:, 0:1])
                if qb == NT - 1 and rb == 2:
                    kw = dict(bias=edge[:, 1:2])
                act.op("activation", pw[:, :, :].rearrange("p c q -> p (c q)"), sb[:, :], AF.Exp, reads=[sb, edge], writes=[pw], **kw)
                for v in range(2):
                    pe.op("matmul", pO[v][:, 0:256].rearrange("p (c q) -> p c q", c=2), lhsT=vaw[:, blk, g, v, :], rhs=pw[:, v::2, :],
                          start=(rb == 0), stop=(rb == 2), reads=[vaw, pw], writes=[pO[v]], sig=(rb == 2))
            den = den_[(qb * 2 + g) % 2]
            for v in range(2):
                np_ = slice(0, 64) if v == 0 else slice(64, 128)
                dp_ = slice(64, 128) if v == 0 else slice(0, 64)
                h0 = g * 4 + v
                dve.op("tensor_tensor", den[dp_, 0, :].rearrange("p (c q) -> p c q", c=2), pO[v][dp_, 0:256].rearrange("p (c q) -> p c q", c=2),
                       sinkE[dp_, h0:h0 + 3:2, :], op=ALU.add, reads=[pO[v], sinkE], writes=[den])
                dve.op("reciprocal", den[np_, 1, :], den[dp_, 0, :], reads=[den], writes=[den])
                ch = 4 + g * 2
                dve.op("tensor_tensor", catT[np_, ch:ch + 2, qs], pO[v][np_, 0:256].rearrange("p (c q) -> p c q", c=2),
                       den[np_, 1, :].rearrange("p (c q) -> p c q", c=2), op=ALU.mult, reads=[pO[v], den], writes=[catT])
    kb.barrier()
    es.close()
    esBD.close()

    es = ExitStack()
    KT = [kb.tile(es, "KT", [96, SEQ], BF16) for _ in range(2)]
    vaug = [kb.tile(es, "vaug", [128, 64, 128], BF16) for _ in range(2)]
    for i in range(2):
        dve.op("memset", vaug[i][:, :, :].rearrange("p a d -> p (a d)"), 1.0, writes=[vaug[i]])
    QT = [kb.tile(es, "QT", [96, TOK], BF16) for _ in range(2)]
    csq = kb.tile(es, "csq", [96, 2, TOK], F32)
    sp.dma(csq[64:96, 0, :], I["cs_own"][0, :, :], d_ld, writes=[csq])
    sp.dma(csq[64:96, 1, :], I["cs_own"][1, :, :], d_ld, writes=[csq])
    t12q = [kb.tile(es, "t12q", [96, 2, 512], F32) for _ in range(2)]
    P_ = [kb.tile(es, "P", [128, 512], BF16) for _ in range(4)]
    rec_ = [kb.tile(es, "rec", [128, 512], F32) for _ in range(2)]
    SB = kb.ps[0:3]
    OB = kb.ps[3:5]
    PB = kb.ps[5:8]
    pbi = [0]

    def pbank():
        b = PB[pbi[0] % 3]
        pbi[0] += 1
        return b

    def prep(h):
        kt, va, qt_ = KT[h % 2], vaug[h % 2], QT[h % 2]
        if h < 2:
            for t in range(4):
                sl = slice(t * 2048, (t + 1) * 2048)
                pool.op("tensor_copy", kt[64:96, sl], kpeT[64:96, sl], reads=[kpeT], writes=[kt])
        for t in range(SEQ // 512):
            sl = slice(t * 512, (t + 1) * 512)
            p = pbank()
            pe.op("matmul", p[0:64, :], lhsT=wukv[:, h * 128:h * 128 + 64], rhs=kvnT[:, sl], start=True, stop=True,
                  reads=[wukv, kvnT], writes=[p])
            dve.op("tensor_copy", kt[0:64, sl], p[0:64, :], reads=[p], writes=[kt])
        off = 0 if h % 2 == 0 else 64
        for k8 in range(8):
            p = pbank()
            for j in range(8):
                kbk = k8 * 8 + j
                pe.op("matmul", p[:, j * 64:(j + 1) * 64], lhsT=kvnT[:, kbk * 128:(kbk + 1) * 128], rhs=wukv[:, h * 128 + 64:h * 128 + 128],
                      start=True, stop=True, reads=[wukv, kvnT], writes=[p], sig=(j == 7))
            dve.op("tensor_copy", va[:, k8 * 8:(k8 + 1) * 8, off:off + 64], p[:, :].rearrange("p (a d) -> p a d", a=8), reads=[p], writes=[va])
        for q in range(4):
            sl = slice(q * 512, (q + 1) * 512)
            p1, p2 = pbank(), pbank()
            for c in range(2):
                pe.op("matmul", p1[0:96, :], lhsT=wuq[:, c, h * 96:(h + 1) * 96], rhs=qnT[:, c, sl], start=(c == 0), stop=(c == 1),
                      reads=[wuq, qnT], writes=[p1], sig=(c == 1))
            for c in range(2):
                pe.op("matmul", p2[0:96, :], lhsT=wuqr[:, c, h, :], rhs=qnT[:, c, sl], start=(c == 0), stop=(c == 1),
                      reads=[wuqr, qnT], writes=[p2], sig=(c == 1))
            tq = t12q[q % 2]
            dve.op("tensor_copy", qt_[0:64, sl], p1[0:64, :], reads=[p1], writes=[qt_])
            dve.op("tensor_tensor", tq[64:96, 0, :], p1[64:96, :], csq[64:96, 0, sl], op=ALU.mult, reads=[p1, csq], writes=[tq])
            dve.op("tensor_tensor", tq[64:96, 1, :], p2[64:96, :], csq[64:96, 1, sl], op=ALU.mult, reads=[p2, csq], writes=[tq])
            dve.op("tensor_tensor", qt_[64:96, sl], tq[64:96, 0, :], tq[64:96, 1, :], op=ALU.add, reads=[tq], writes=[qt_])

    mscale = 96 ** -0.5
    cnt = [0, 0]
    NKB = SEQ // 128
    prep(0)
    for h in range(8):
        if h + 1 < 8:
            prep(h + 1)
        kt, va, qt_ = KT[h % 2], vaug[h % 2], QT[h % 2]
        np_ = slice(0, 64) if h % 2 == 0 else slice(64, 128)
        dp_ = slice(64, 128) if h % 2 == 0 else slice(0, 64)
        for q in range(4):
            sl = slice(q * 512, (q + 1) * 512)
            pO = OB[cnt[1] % 2]
            cnt[1] += 1

            def S(kbk):
                ps = SB[kbk % 3]
                pe.op("matmul", ps[:, :], lhsT=kt[:, kbk * 128:(kbk + 1) * 128], rhs=qt_[:, sl], start=True, stop=True,
                      reads=[kt, qt_], writes=[ps])
            S(0)
            S(1)
            for kbk in range(NKB):
                ps = SB[kbk % 3]
                P = P_[kbk % 4]
                act.op("activation", P[:, :], ps[:, :], AF.Exp, scale=mscale, reads=[ps], writes=[P])
                pe.op("matmul", pO[:, :], lhsT=va[:, kbk, :], rhs=P[:, :], start=(kbk == 0), stop=(kbk == NKB - 1),
                      reads=[va, P], writes=[pO], sig=(kbk == NKB - 1))
                if kbk + 2 < NKB:
                    S(kbk + 2)
            rec = rec_[cnt[1] % 2]
            dve.op("reciprocal", rec[np_, :], pO[dp_, :], reads=[pO], writes=[rec])
            dve.op("tensor_tensor", catT[np_, h // 2, sl], pO[np_, :], rec[np_, :], op=ALU.mult, reads=[pO, rec], writes=[catT])
    kb.barrier()
    es.close()
    esP.close()


def emit_outproj_ln(kb, I, srcT, wname, res_dram, acc, accb, lnp, ln_idx, rw_t, sc_t, hT, d_ld):
    pe, act, dve, pool, sp = kb.pe, kb.act, kb.dve, kb.pool, kb.sp
    es = ExitStack()
    wout = kb.tile(es, "wout", [128, 8, 1024], BF16)
    pool.dma(wout[:, :, :], I[wname].rearrange("(k p) n -> p k n", p=128), d_ld, writes=[wout])
    xr = [kb.tile(es, "xres", [128, 1024], F32) for _ in range(2)]
    xds = [kb.dsem(f"d_xres{ln_idx}_{i}") for i in range(2)]
    for tt in range(NT):
        x_t = xr[tt % 2]
        sp.dma(x_t[:, :], res_dram[tt * 128:(tt + 1) * 128, :], xds[tt % 2], writes=[x_t])
        for half in range(2):
            ps = kb.bank()
            hs = slice(half * 512, (half + 1) * 512)
            for k in range(8):
                pe.op("matmul", ps[:, :], lhsT=srcT[:, k, tt * 128:(tt + 1) * 128], rhs=wout[:, k, hs], start=(k == 0), stop=(k == 7),
                      reads=[srcT, wout], writes=[ps], sig=(k == 7))
            dve.op("scalar_tensor_tensor", acc[:, tt, hs], x_t[:, hs], float(ALPHA), ps[:, :], op0=ALU.mult, op1=ALU.add,
                   reads=[x_t, ps], writes=[accb[tt]])
    emit_ln(kb, es, acc, accb, lnp, ln_idx, True, rw_t, sc_t, hT, None, ALPHA, d_ld)
    kb.barrier()
    es.close()


def emit_l1_mixer(kb, I, hT, g, mode, summ_out):
    pe, act, dve, pool, sp = kb.pe, kb.act, kb.dve, kb.pool, kb.sp
    d_ld = kb.dsem("d_l1ld")
    es = ExitStack()
    xc = kb.tile(es, "xc", [128, 8, TOK], F32)
    xcb = [Buf() for _ in range(8)]
    gb = [Buf() for _ in range(8)]
    cw = kb.tile(es, "cw", [128, 8, 4], F32)
    cb = kb.tile(es, "cb", [128, 8], F32)
    bv = kb.tile(es, "bv", [128, 48], F32)
    msk = kb.tile(es, "msk", [128, 8], F32)
    sp.dma(cw[:, :, :], I["convw"][:, :, :], d_ld, writes=[cw])
    sp.dma(cb[:, :], I["convb"][:, :], d_ld, writes=[cb])
    sp.dma(bv[:, :], I["bvec"][:, :], d_ld, writes=[bv])
    sp.dma(msk[:, :], I["scan_mask"][:, :], d_ld, writes=[msk])
    wl = {}
    for nm in ("l1_wa_f", "l1_wx_f", "l1_wa_b", "l1_wx_b"):
        wl[nm] = kb.tile(es, nm, [128, 8, 128], BF16)
        pool.dma(wl[nm][:, :, :], I[nm].rearrange("n c d -> c n d"), d_ld, writes=[wl[nm]])
    halo = kb.tile(es, "haloT", [128, 8, 4], BF16)
    with kb.nc.allow_non_contiguous_dma(reason="tiny halo load"):
        pool.dma(halo[:, :, :], I["h_haloT"].rearrange("(k p) c -> p k c", p=128), d_ld, writes=[halo])
    cl = kb.tile(es, "cl", [128, 16], F32)
    cl2 = kb.tile(es, "cl2", [128, 16], F32)
    for d in range(2):
        lam = bv[:, d * 24 + 16:d * 24 + 24]
        act.op("activation", cl[:, d * 8:(d + 1) * 8], lam, AF.Exp, scale=-1.0, reads=[bv], writes=[cl])
    dve.op("tensor_scalar", cl2[:, :], cl[:, :], -0.25, 1.0 / 3, op0=ALU.mult, op1=ALU.add, reads=[cl], writes=[cl2])
    dve.op("tensor_tensor", cl2[:, :], cl2[:, :], cl[:, :], op=ALU.mult, reads=[cl2, cl], writes=[cl2])
    dve.op("tensor_scalar", cl2[:, :], cl2[:, :], -0.5, None, op0=ALU.add, reads=[cl2], writes=[cl2])
    dve.op("tensor_tensor", cl2[:, :], cl2[:, :], cl[:, :], op=ALU.mult, reads=[cl2, cl], writes=[cl2])
    dve.op("tensor_scalar", cl2[:, :], cl2[:, :], 1.0, None, op0=ALU.add, reads=[cl2], writes=[cl2])
    dve.op("tensor_tensor", cl[:, :], cl2[:, :], cl[:, :], op=ALU.mult, reads=[cl2, cl], writes=[cl])
    dve.op("tensor_scalar", cl2[:, :], cl[:, :], -16.0, None, op0=ALU.mult, reads=[cl], writes=[cl2])
    dve.op("tensor_scalar", cl[:, :], cl[:, :], -8.0, None, op0=ALU.mult, reads=[cl], writes=[cl])

    esG = ExitStack()
    wg_ = [kb.tile(esG, "wg", [128, 8, 128], BF16) for _ in range(2)]
    wx_ = [kb.tile(esG, "wx", [128, 8, 128], BF16) for _ in range(2)]
    wds = [kb.dsem(f"d_w1c{i}") for i in range(2)]
    xr_ = [kb.tile(esG, "xrext", [128, TOK + 4], F32) for _ in range(2)]
    w1v = I["l1_w_in"].rearrange("(k p) n -> p k n", p=128)
    for n in range(8):
        wg, wx, xr = wg_[n % 2], wx_[n % 2], xr_[n % 2]
        pool.dma(wg[:, :, :], w1v[:, :, n * 128:(n + 1) * 128], wds[n % 2], writes=[wg])
        pool.dma(wx[:, :, :], w1v[:, :, 1024 + n * 128:1024 + (n + 1) * 128], wds[n % 2], writes=[wx])
        for t in range(4):
            sl = slice(t * 512, (t + 1) * 512)
            pg, px = kb.bank(), kb.bank()
            for k in range(8):
                pe.op("matmul", pg[:, :], lhsT=wg[:, k, :], rhs=hT[:, k, sl], start=(k == 0), stop=(k == 7),
                      reads=[wg] + hT.tb[t * 4:t * 4 + 4], writes=[pg], sig=(k == 7))
            for k in range(8):
                pe.op("matmul", px[:, :], lhsT=wx[:, k, :], rhs=hT[:, k, sl], start=(k == 0), stop=(k == 7),
                      reads=[wx] + hT.tb[t * 4:t * 4 + 4], writes=[px], sig=(k == 7))
            act.op("activation", g[:, n, sl], pg[:, :], AF.Gelu_apprx_tanh, reads=[pg], writes=[gb[n]])
            act.op("copy", xr[:, 2 + t * 512:2 + (t + 1) * 512], px[:, :], reads=[px], writes=[xr])
        ph = kb.bank()
        for k in range(8):
            pe.op("matmul", ph[:, 0:4], lhsT=wx[:, k, :], rhs=halo[:, k, :], start=(k == 0), stop=(k == 7),
                  reads=[wx, halo], writes=[ph], sig=(k == 7))
        act.op("copy", xr[:, 0:2], ph[:, 0:2], reads=[ph], writes=[xr])
        act.op("copy", xr[:, TOK + 2:TOK + 3], ph[:, 2:3], reads=[ph], writes=[xr])
        dve.op("tensor_scalar", xc[:, n, :], xr[:, 0:TOK], cw[:, n, 0:1], cb[:, n:n + 1], op0=ALU.mult, op1=ALU.add,
               reads=[xr, cw, cb], writes=[xcb[n]])
        for k in range(1, 4):
            dve.op("scalar_tensor_tensor", xc[:, n, :], xr[:, k:k + TOK], cw[:, n, k:k + 1], xc[:, n, :], op0=ALU.mult, op1=ALU.add,
                   reads=[xr, cw, xcb[n]], writes=[xcb[n]])
    kb.barrier()
    esG.close()

    esS = ExitStack()
    x16_ = [kb.tile(esS, "x16", [128, TOK], BF16) for _ in range(1)]
    r_ = [kb.tile(esS, "r", [128, TOK], F32) for _ in range(1)]
    i_ = [kb.tile(esS, "ig", [128, TOK], F32) for _ in range(1)]
    a_t = kb.tile(esS, "a", [128, TOK], F32)
    s_t = kb.tile(esS, "sq", [128, TOK], F32)
    b_t = kb.tile(esS, "b", [128, TOK], F32)
    hd = [kb.tile(esS, "hf", [128, TOK], F32), kb.tile(esS, "hb", [128, TOK], F32)]
    summ = kb.tile(esS, "summ", [128, 32], F32)
    sall = kb.tile(esS, "sall", [128, 4, 32], F32)
    hin = kb.tile(esS, "hin", [128, 16], F32)
    tmp8 = [kb.tile(esS, "tmp8", [128, 8], F32) for _ in range(3)]
    wnames = [("l1_wa_f", "l1_wx_f"), ("l1_wa_b", "l1_wx_b")]
    cnt = [0]

    def scan_pass(n, d, x16, init, want_sum):
        r, ig = r_[0], i_[0]
        cnt[0] += 1
        wa, wx = wl[wnames[d][0]], wl[wnames[d][1]]
        for t in range(4):
            sl = slice(t * 512, (t + 1) * 512)
            pr, pi = kb.bank(), kb.bank()
            pe.op("matmul", pr[:, :], lhsT=wa[:, n, :], rhs=x16[:, sl], start=True, stop=True, reads=[wa, x16], writes=[pr])
            pe.op("matmul", pi[:, :], lhsT=wx[:, n, :], rhs=x16[:, sl], start=True, stop=True, reads=[wx, x16], writes=[pi])
            act.op("activation", r[:, sl], pr[:, :], AF.Sigmoid, bias=bv[:, d * 24 + n:d * 24 + n + 1], scale=1.0, reads=[pr, bv], writes=[r])
            act.op("activation", ig[:, sl], pi[:, :], AF.Sigmoid, bias=bv[:, d * 24 + 8 + n:d * 24 + 9 + n], scale=1.0, reads=[pi, bv], writes=[ig])
        if want_sum:
            dve.op("tensor_reduce", summ[:, d * 16 + n:d * 16 + n + 1], r[:, :], axis=AX.X, op=ALU.add, reads=[r], writes=[summ])
        c = d * 8 + n
        act.op("activation", a_t[:, :], r[:, :], AF.Exp, scale=cl[:, c:c + 1], reads=[r, cl], writes=[a_t])
        act.op("activation", s_t[:, :], r[:, :], AF.Exp, scale=cl2[:, c:c + 1], reads=[r, cl2], writes=[s_t])
        act.op("activation", s_t[:, :], s_t[:, :], AF.Sqrt, scale=-1.0, bias=1.0, reads=[s_t], writes=[s_t])
        dve.op("tensor_tensor", b_t[:, :], ig[:, :], xc[:, n, :], op=ALU.mult, reads=[ig, xcb[n]], writes=[b_t])
        dve.op("tensor_tensor", b_t[:, :], b_t[:, :], s_t[:, :], op=ALU.mult, reads=[b_t, s_t], writes=[b_t])
        h = hd[d]
        ini = 0.0 if init is None else init
        rd = [a_t, b_t] + ([hin] if init is not None else [])
        if d == 0:
            dve.op("tensor_tensor_scan", h[:, :], a_t[:, :], b_t[:, :], ini, op0=ALU.mult, op1=ALU.add, reads=rd, writes=[h])
        else:
            dve.op("tensor_tensor_scan", h[:, ::-1], a_t[:, ::-1], b_t[:, ::-1], ini, op0=ALU.mult, op1=ALU.add, reads=rd, writes=[h])
        return h

    def mk16(n):
        x16 = x16_[0]
        act.op("copy", x16[:, :], xc[:, n, :], reads=[xcb[n]], writes=[x16])
        return x16

    if mode in ("L1A", "FUSED"):
        for n in range(8):
            x16 = mk16(n)
            for d in range(2):
                h = scan_pass(n, d, x16, None, True)
                src = h[:, TOK - 1:TOK] if d == 0 else h[:, 0:1]
                dve.op("tensor_copy", summ[:, d * 16 + 8 + n:d * 16 + 9 + n], src, reads=[h], writes=[summ])
    if mode == "L1A":
        d_o = kb.dsem("d_summ")
        sp.dma(summ_out[:, :], summ[:, :], d_o, reads=[summ])
        kb.barrier()
        esS.close()
        es.close()
        return
    if mode == "L1B":
        sp.dma(sall[:, :, :], I["summ_all"].rearrange("j p c -> p j c"), d_ld, writes=[sall])
    t_a, t_s, t_n = tmp8
    for d in range(2):
        sv = hin[:, d * 8:(d + 1) * 8]
        dve.op("memset", sv, 0.0, writes=[hin])
        order = range(4) if d == 0 else range(3, -1, -1)
        for jj in order:
            dve.op("tensor_tensor", t_a[:, :], sall[:, jj, d * 16:d * 16 + 8], cl[:, d * 8:(d + 1) * 8], op=ALU.mult, reads=[sall, cl], writes=[t_a])
            act.op("activation", t_a[:, :], t_a[:, :], AF.Exp, reads=[t_a], writes=[t_a])
            dve.op("tensor_tensor", t_n[:, :], t_a[:, :], sv, op=ALU.mult, reads=[t_a, hin], writes=[t_n])
            dve.op("tensor_tensor", t_n[:, :], t_n[:, :], sall[:, jj, d * 16 + 8:d * 16 + 16], op=ALU.add, reads=[t_n, sall], writes=[t_n])
            dve.op("tensor_tensor", t_n[:, :], t_n[:, :], sv, op=ALU.subtract, reads=[t_n, hin], writes=[t_n])
            mc = d * 4 + jj
            dve.op("scalar_tensor_tensor", sv, t_n[:, :], msk[:, mc:mc + 1], sv, op0=ALU.mult, op1=ALU.add, reads=[t_n, msk, hin], writes=[hin])
    for n in range(8):
        x16 = mk16(n)
        hf = scan_pass(n, 0, x16, hin[:, n:n + 1], False)
        hb = scan_pass(n, 1, x16, hin[:, 8 + n:9 + n], False)
        dve.op("tensor_tensor", hf[:, :], hf[:, :], hb[:, :], op=ALU.add, reads=[hf, hb], writes=[hf])
        dve.op("tensor_tensor", g[:, n, :], hf[:, :], g[:, n, :], op=ALU.mult, reads=[hf, gb[n]], writes=[gb[n]])
    kb.barrier()
    esS.close()
    es.close()


def build(mode, debug=False):
    nc = bass.Bass("TRN2", target_bir_lowering=False)
    I = {}
    if debug:
        dbg_cat = nc.dram_tensor("dbg_cat", [128, 8, TOK], F32, kind="ExternalOutput").ap()
        dbg_h1 = nc.dram_tensor("dbg_h1", [128, NT, 1024], F32, kind="ExternalOutput").ap()
        dbg_moe = nc.dram_tensor("dbg_moe", [128, NT, 1024], F32, kind="ExternalOutput").ap()

    def inp(name, shape):
        I[name] = dram_in(nc, name, shape)
    inp("ident", [128, 128]); inp("ln_tok", [4, 2, 128, 1024]); inp("ln_fm", [4, 128, 16])
    inp("rw", [128, 8, 16]); inp("rb", [128, 256])
    if mode == "L0":
        inp("xT_full", [D, SEQ]); inp("xT_own", [D, TOK]); inp("xT_halo", [D, 256]); inp("x_own", [TOK, D])
        inp("cs_full", [2, 32, SEQ]); inp("cs_own", [2, 32, TOK]); inp("swa_bias", [128, 3, 8, 128]); inp("swa_mask", [128, 3, 8, 128])
        inp("edge", [128, 2]); inp("l0_w_in", [D, 1184]); inp("qn_g", [128, 2]); inp("kvn_g", [128, 1])
        inp("l0_w_uq", [256, 768]); inp("l0_w_ukv", [128, 1024]); inp("sinks_b", [128, 8]); inp("l0_w_out", [D, D])
        inp("l0_w1", [16, D, 512]); inp("l0_w3", [16, D, 512]); inp("l0_w2", [16, 512, D])
    if mode in ("L1A", "L1B"):
        inp("h_in", [TOK, D]); inp("h_haloT", [D, 4]); inp("l1_w_in", [D, 2048]); inp("convw", [128, 8, 4]); inp("convb", [128, 8])
        inp("bvec", [128, 48]); inp("scan_mask", [128, 8])
        for nm in ("l1_wa_f", "l1_wx_f", "l1_wa_b", "l1_wx_b"):
            inp(nm, [8, 128, 128])
    if mode == "L1B":
        inp("summ_all", [4, 128, 32]); inp("l1_w_out", [D, D])
        inp("l1_w1", [16, D, 512]); inp("l1_w3", [16, D, 512]); inp("l1_w2", [16, 512, D])
    if mode == "L1A":
        out = nc.dram_tensor("summ", [128, 32], F32, kind="ExternalOutput").ap()
    else:
        out = nc.dram_tensor("out", [TOK, D], F32, kind="ExternalOutput").ap()
    kb = KB(nc)
    pe, act, dve, pool, sp = kb.pe, kb.act, kb.dve, kb.pool, kb.sp
    with kb.es:
        d_ld = kb.dsem("d_ld")
        esG = ExitStack()
        ident = kb.tile(esG, "ident", [128, 128], F32)
        sp.dma(ident[:, :], I["ident"][:, :], d_ld, writes=[ident])
        rw_t = kb.tile(esG, "rw", [128, 8, 16], F32)
        sp.dma(rw_t[:, :, :], I["rw"][:, :, :], d_ld, writes=[rw_t])
        rb_t = kb.tile(esG, "rb", [128, 256], F32)
        sp.dma(rb_t[:, :], I["rb"][:, :], d_ld, writes=[rb_t])
        sc_t = kb.tile(esG, "sc", [128, NT, 16], F32)
        lnp = (I["ln_tok"], I["ln_fm"], ident)
        if mode == "L0":
            esC = ExitStack()
            catT = kb.tile(esC, "catT", [128, 8, TOK], BF16)
            emit_l0_mixer(kb, I, catT)
            esL = ExitStack()
            acc = kb.tile(esL, "acc", [128, NT, 1024], F32)
            accb = [Buf() for _ in range(NT)]
            hT = kb.tile(esL, "hT", [128, 8, TOK], BF16)
            hT.tb = [Buf() for _ in range(NT)]
            emit_outproj_ln(kb, I, catT, "l0_w_out", I["x_own"], acc, accb, lnp, 0, rw_t, sc_t, hT, d_ld)
            if debug:
                d_dbg = kb.dsem("d_dbg")
                pool.dma(dbg_cat[:, :, :], catT[:, :, :], d_dbg, reads=[catT])
                pool.dma(dbg_h1[:, :, :], acc[:, :, :], d_dbg, reads=accb)
                kb.barrier()
            esM = ExitStack()
            emit_moe(kb, esM, acc, accb, hT, sc_t, rb_t, I["l0_w1"], I["l0_w3"], I["l0_w2"], d_ld)
            if debug:
                pool.dma(dbg_moe[:, :, :], acc[:, :, :], d_dbg, reads=accb)
            kb.barrier()
            esM.close()
            esN = ExitStack()
            emit_ln(kb, esN, acc, accb, lnp, 1, False, rw_t, sc_t, None, out, 1.0, d_ld)
            kb.barrier()
            esN.close()
            esL.close()
            esC.close()
        if mode in ("L1A", "L1B"):
            esL = ExitStack()
            hT = kb.tile(esL, "hT", [128, 8, TOK], BF16)
            hT.tb = [Buf() for _ in range(NT)]
            esT = ExitStack()
            xt_ = [kb.tile(esT, "xin", [128, 1024], F32) for _ in range(2)]
            xtd = [kb.dsem(f"d_xin{i}") for i in range(2)]
            for tt in range(NT):
                x_t = xt_[tt % 2]
                sp.dma(x_t[:, :], I["h_in"][tt * 128:(tt + 1) * 128, :], xtd[tt % 2], writes=[x_t])
                for half in range(2):
                    ps = kb.bank()
                    for j in range(4):
                        k = half * 4 + j
                        pe.op("transpose", ps[:, j * 128:(j + 1) * 128], x_t[:, k * 128:(k + 1) * 128], ident[:, :],
                              reads=[x_t, ident], writes=[ps], sig=(j == 3))
                    act.op("copy", hT[:, half * 4:(half + 1) * 4, tt * 128:(tt + 1) * 128], ps[:, :].rearrange("p (a b) -> p a b", a=4),
                           reads=[ps], writes=[hT.tb[tt]])
            kb.barrier()
            esT.close()
            esY = ExitStack()
            g = kb.tile(esY, "g", [128, 8, TOK], BF16)
            emit_l1_mixer(kb, I, hT, g, mode, out)
            if mode == "L1B":
                esA = ExitStack()
                acc = kb.tile(esA, "acc", [128, NT, 1024], F32)
                accb = [Buf() for _ in range(NT)]
                emit_outproj_ln(kb, I, g, "l1_w_out", I["h_in"], acc, accb, lnp, 2, rw_t, sc_t, hT, d_ld)
                esM = ExitStack()
                emit_moe(kb, esM, acc, accb, hT, sc_t, rb_t, I["l1_w1"], I["l1_w3"], I["l1_w2"], d_ld)
                kb.barrier()
                esM.close()
                esN = ExitStack()
                emit_ln(kb, esN, acc, accb, lnp, 3, False, rw_t, sc_t, None, out, 1.0, d_ld)
                kb.barrier()
                esN.close()
                esA.close()
            esY.close()
            esL.close()
        kb.barrier()
        esG.close()
    return nc


def _t5_bucket(rel):
    n_side, max_exact = 16, 8
    dist = np.abs(rel)
    far = max_exact + (np.log(np.maximum(dist, 1).astype(np.float32) / max_exact) / math.log(128 / max_exact) * (n_side - max_exact)).astype(np.int32)
    far = np.minimum(far, n_side - 1)
    return np.where(rel > 0, n_side, 0) + np.where(dist < max_exact, dist, far)


def _consts():
    half = 16
    inv_freq = (10000.0 ** (-np.arange(half, dtype=np.float32) / half)).astype(np.float32)
    ang = np.arange(SEQ, dtype=np.float32)[:, None] * inv_freq[None, :]
    cos, sin = np.cos(ang).astype(np.float32), np.sin(ang).astype(np.float32)
    cs = np.stack([np.concatenate([cos.T, cos.T], 0), np.concatenate([sin.T, sin.T], 0)], 0)
    k_in = np.arange(128)[:, None, None]
    rbk = np.arange(3)[None, :, None]
    q = np.arange(128)[None, None, :]
    rel = rbk * 128 + k_in - 128 - q
    bucket = _t5_bucket(rel)
    mask = np.where(np.abs(rel) <= 128, 0.0, NEG).astype(np.float32)
    mask = np.ascontiguousarray(np.broadcast_to(mask[:, :, None, :], (128, 3, 8, 128)))
    return np.ascontiguousarray(cs), bucket, mask


def _bc(v, n=128):
    return np.ascontiguousarray(np.broadcast_to(np.asarray(v, np.float32).reshape(1, -1), (n, np.asarray(v).size)))


def _fm(v, k):
    return np.ascontiguousarray(np.asarray(v, np.float32).reshape(k, 128).T)


def common_maps(inputs):
    ln_names = [("l0_ln1_g", "l0_ln1_b"), ("l0_ln2_g", "l0_ln2_b"), ("l1_ln1_g", "l1_ln1_b"), ("l1_ln2_g", "l1_ln2_b")]
    ln_tok = np.stack([np.stack([_bc(inputs[g]), _bc(inputs[b])]) for g, b in ln_names]).astype(np.float32)
    ln_fm = np.stack([np.concatenate([_fm(inputs[g], 8), _fm(inputs[b], 8)], 1) for g, b in ln_names]).astype(np.float32)
    rw = np.ascontiguousarray(np.asarray(inputs["router_w"], np.float32).reshape(8, 128, 16).transpose(1, 0, 2))
    rb = _bc(np.tile(np.asarray(inputs["router_bias"], np.float32), NT))
    return dict(ident=np.eye(128, dtype=np.float32), ln_tok=ln_tok, ln_fm=ln_fm, rw=rw, rb=rb)


def l0_maps(inputs):
    x = np.asarray(inputs["x"], np.float32)
    cs, bucket, mask = _consts()
    rel_bias = np.asarray(inputs["rel_bias"], np.float32)
    swa_bias = np.ascontiguousarray(rel_bias[bucket])
    swa_bias = np.ascontiguousarray(swa_bias.transpose(0, 1, 3, 2))
    w_in = np.asarray(inputs["l0_w_in"], np.float32)
    perm = np.arange(1184)
    qcols = []
    for c in range(4):
        qcols += list(range(416 + c * 64, 416 + (c + 1) * 64)) + list(range(416 + (c + 4) * 64, 416 + (c + 5) * 64))
    perm[416:928] = np.array(qcols)
    w_in_p = np.ascontiguousarray(w_in[:, perm])
    cm = common_maps(inputs)
    shared = dict(cm, swa_bias=swa_bias, swa_mask=mask, l0_w_in=w_in_p, qn_g=_fm(inputs["l0_q_norm"], 2), kvn_g=_fm(inputs["l0_kv_norm"], 1),
                  l0_w_uq=np.asarray(inputs["l0_w_uq"], np.float32), l0_w_ukv=np.asarray(inputs["l0_w_ukv"], np.float32),
                  sinks_b=_bc(inputs["l0_sinks"]), l0_w_out=np.asarray(inputs["l0_w_out"], np.float32),
                  l0_w1=np.asarray(inputs["l0_w1"], np.float32), l0_w3=np.asarray(inputs["l0_w3"], np.float32),
                  l0_w2=np.asarray(inputs["l0_w2"], np.float32), cs_full=cs)
    xT = [np.ascontiguousarray(x[b].T) for b in range(2)]
    maps = []
    for c in range(NCORES):
        b, j = c // 4, c % 4
        s0 = j * TOK
        halo = np.zeros((D, 256), np.float32)
        if j > 0:
            halo[:, 0:128] = xT[b][:, s0 - 128:s0]
        if j < 3:
            halo[:, 128:256] = xT[b][:, s0 + TOK:s0 + TOK + 128]
        edge = np.zeros((128, 2), np.float32)
        if j == 0:
            edge[:, 0] = NEG
        if j == 3:
            edge[:, 1] = NEG
        maps.append(dict(shared, xT_full=xT[b], xT_own=np.ascontiguousarray(xT[b][:, s0:s0 + TOK]), xT_halo=halo,
                         x_own=np.ascontiguousarray(x[b, s0:s0 + TOK]), cs_own=np.ascontiguousarray(cs[:, :, s0:s0 + TOK]), edge=edge))
    return maps


def run_l0(inputs, debug=False):
    nc = build("L0", debug)
    res = run_bass_kernel_spmd(nc, l0_maps(inputs), core_ids=list(range(NCORES)))
    h2 = np.stack([r["out"] for r in res.results]).reshape(2, SEQ, D)
    if debug:
        return h2, res.results
    return h2


def l1_maps(inputs, h2, summ=None):
    cm = common_maps(inputs)
    f = lambda k: np.asarray(inputs[k], np.float32)
    convw = np.ascontiguousarray(f("l1_conv_w").reshape(4, 8, 128).transpose(2, 1, 0))
    convb = _fm(inputs["l1_conv_b"], 8)
    bvec = np.concatenate([np.ascontiguousarray(f("l1_ba_f").T), np.ascontiguousarray(f("l1_bx_f").T), _fm(inputs["l1_lam_f"], 8),
                           np.ascontiguousarray(f("l1_ba_b").T), np.ascontiguousarray(f("l1_bx_b").T), _fm(inputs["l1_lam_b"], 8)], 1)
    shared = dict(cm, l1_w_in=f("l1_w_in"), convw=convw, convb=convb, bvec=np.ascontiguousarray(bvec),
                  l1_wa_f=f("l1_wa_f"), l1_wx_f=f("l1_wx_f"), l1_wa_b=f("l1_wa_b"), l1_wx_b=f("l1_wx_b"))
    if summ is not None:
        shared.update(l1_w_out=f("l1_w_out"), l1_w1=f("l1_w1"), l1_w3=f("l1_w3"), l1_w2=f("l1_w2"))
    maps = []
    for c in range(NCORES):
        b, j = c // 4, c % 4
        s0 = j * TOK
        halo = np.zeros((D, 4), np.float32)
        if j > 0:
            halo[:, 0] = h2[b, s0 - 2]
            halo[:, 1] = h2[b, s0 - 1]
        if j < 3:
            halo[:, 2] = h2[b, s0 + TOK]
        msk = np.zeros((128, 8), np.float32)
        for jj in range(4):
            msk[:, jj] = 1.0 if jj < j else 0.0
            msk[:, 4 + jj] = 1.0 if jj > j else 0.0
        m = dict(shared, h_in=np.ascontiguousarray(h2[b, s0:s0 + TOK]), h_haloT=halo, scan_mask=msk)
        if summ is not None:
            m["summ_all"] = np.ascontiguousarray(np.stack([summ[b * 4 + jj] for jj in range(4)]))
        maps.append(m)
    return maps


def run_l1(inputs, h2):
    nca = build("L1A")
    ra = run_bass_kernel_spmd(nca, l1_maps(inputs, h2), core_ids=list(range(NCORES)))
    summ = [r["summ"] for r in ra.results]
    ncb = build("L1B")
    rb = run_bass_kernel_spmd(ncb, l1_maps(inputs, h2, summ), core_ids=list(range(NCORES)))
    return np.stack([r["out"] for r in rb.results]).reshape(2, SEQ, D)


def kernel(**inputs):
    h2 = run_l0(inputs)
    out = run_l1(inputs, h2)
    return out.astype(np.float32)
```
